# Optimizing a Trainium2 kernel written in Bass

```python
import math
import jax
import jax.numpy as jnp
from jax import lax
import numpy as np

D_MODEL = 2048
BATCH = 16
SEQ = 2048
DEPTH = 4

MEM_LEN = 256
EPS = 1e-6
ROPE_THETA = 10000.0
POOL_WINDOWS = (2, 4, 8, 16)
POOL_GROUPS = len(POOL_WINDOWS)
POOL_GC = D_MODEL // POOL_GROUPS
N_HEADS = 16
HEAD_DIM = D_MODEL // N_HEADS
IDX_HEADS = 16
IDX_DIM = 64
TOPK_MAX = 256
Q_BLOCK = 128
X_HEADS = 4
X_HEAD_DIM = 128
FFN_HIDDEN = ((8 * D_MODEL + 3 * 256 - 1) // (3 * 256)) * 256
ATTN_SPLITS = (N_HEADS * HEAD_DIM, HEAD_DIM, HEAD_DIM, IDX_HEADS * IDX_DIM, IDX_DIM, IDX_HEADS)
ATTN_IN = sum(ATTN_SPLITS)
N_POOL_LAYERS = (DEPTH + 1) // 2
N_ATTN_LAYERS = DEPTH // 2

kernel_name = 'hybrid_pool_dsa_memxattn_trunk'


def rms_norm(x, g):
    xf = x.astype(jnp.float32)
    y = xf * lax.rsqrt(jnp.mean(xf * xf, axis=-1, keepdims=True) + EPS)
    return (y * g.astype(jnp.float32)).astype(x.dtype)


def rope_tables(positions, dim):
    inv_freq = ROPE_THETA ** (-(jnp.arange(0, dim, 2, dtype=jnp.float32) / dim))
    ang = positions.astype(jnp.float32)[..., None] * inv_freq
    return jnp.cos(ang), jnp.sin(ang)


def apply_rope(x, cos, sin):
    xf = x.astype(jnp.float32)
    x1, x2 = jnp.split(xf, 2, axis=-1)
    out = jnp.concatenate([x1 * cos - x2 * sin, x2 * cos + x1 * sin], axis=-1)
    return out.astype(x.dtype)


def pool_mixer(h, w, scale):
    B, S, D = h.shape
    hg = h.reshape(B, S, POOL_GROUPS, POOL_GC).astype(jnp.float32)
    c = jnp.concatenate([jnp.zeros((B, 1, POOL_GROUPS, POOL_GC), jnp.float32),
                         jnp.cumsum(hg, axis=1)], axis=1)
    t = jnp.arange(S)
    means = []
    for g, win in enumerate(POOL_WINDOWS):
        lo = jnp.maximum(t + 1 - win, 0)
        cnt = (t + 1 - lo).astype(jnp.float32)
        means.append((c[:, 1:, g] - c[:, lo, g]) / cnt[None, :, None])
    diff = (jnp.stack(means, axis=2) - hg).astype(h.dtype)
    y = jnp.einsum('bsgc,gcd->bsgd', diff, w).reshape(B, S, D)
    return y * scale


def dsa_attention(h, w_in, w_out, cos_a, sin_a, cos_i, sin_i):
    B, S, _ = h.shape
    proj = h @ w_in
    bounds = list(np.cumsum(ATTN_SPLITS)[:-1])
    q, k, v, qi, ki, wi = jnp.split(proj, bounds, axis=-1)
    q = apply_rope(q.reshape(B, S, N_HEADS, HEAD_DIM), cos_a[:, :, None, :], sin_a[:, :, None, :])
    k = apply_rope(k, cos_a, sin_a)
    qi = apply_rope(qi.reshape(B, S, IDX_HEADS, IDX_DIM), cos_i[:, :, None, :], sin_i[:, :, None, :])
    ki = apply_rope(ki, cos_i, sin_i)
    wi = wi * (IDX_HEADS ** -0.5 * IDX_DIM ** -0.5)
    topk = min(TOPK_MAX, S // 4)
    n_blocks = S // Q_BLOCK
    key_pos = jnp.arange(S)
    att_scale = HEAD_DIM ** -0.5
    gather = jax.vmap(lambda src, ids: src[ids])

    def block(start):
        q_b = lax.dynamic_slice_in_dim(q, start, Q_BLOCK, axis=1)
        qi_b = lax.dynamic_slice_in_dim(qi, start, Q_BLOCK, axis=1)
        wi_b = lax.dynamic_slice_in_dim(wi, start, Q_BLOCK, axis=1)
        q_pos = start + jnp.arange(Q_BLOCK)
        causal = key_pos[None, :] <= q_pos[:, None]
        rel = jax.nn.relu(jnp.einsum('bqhd,bsd->bqhs', qi_b, ki))
        iscore = jnp.einsum('bqh,bqhs->bqs', wi_b, rel).astype(jnp.float32)
        iscore = jnp.where(causal[None], iscore, -jnp.inf)
        _, idx = lax.top_k(iscore, topk)
        valid = idx <= q_pos[None, :, None]
        k_sel = gather(k, idx)
        v_sel = gather(v, idx)
        logits = jnp.einsum('bqhd,bqkd->bqhk', q_b, k_sel).astype(jnp.float32) * att_scale
        logits = jnp.where(valid[:, :, None, :], logits, -jnp.inf)
        p = jax.nn.softmax(logits, axis=-1).astype(v_sel.dtype)
        return jnp.einsum('bqhk,bqkd->bqhd', p, v_sel)

    out = lax.map(block, jnp.arange(n_blocks) * Q_BLOCK)
    out = jnp.moveaxis(out, 0, 1).reshape(B, S, N_HEADS * HEAD_DIM)
    return out @ w_out


def memory_cross_attention(h, memn, w_q, w_kv, w_o):
    B, S, _ = h.shape
    M = memn.shape[1]
    q = (h @ w_q).reshape(B, S, X_HEADS, X_HEAD_DIM)
    kv = (memn @ w_kv).reshape(B, M, 2, X_HEADS, X_HEAD_DIM)
    k, v = kv[:, :, 0], kv[:, :, 1]
    logits = jnp.einsum('bshd,bmhd->bhsm', q, k).astype(jnp.float32) * (X_HEAD_DIM ** -0.5)
    p = jax.nn.softmax(logits, axis=-1).astype(v.dtype)
    out = jnp.einsum('bhsm,bmhd->bshd', p, v).reshape(B, S, X_HEADS * X_HEAD_DIM)
    return out @ w_o


def swiglu(h, w_in, w_out):
    g, u = jnp.split(h @ w_in, 2, axis=-1)
    return (jax.nn.silu(g) * u) @ w_out


def setup_inputs(seed: int = 0) -> dict:
    key = jax.random.key(seed)
    ks = jax.random.split(key, 17)

    def nrm(k, shape, fan_in):
        return jax.random.normal(k, shape, jnp.float32) * (fan_in ** -0.5)

    def gain(k, shape):
        return 1.0 + 0.02 * jax.random.normal(k, shape, jnp.float32)

    x = jax.random.normal(ks[0], (BATCH, SEQ, D_MODEL), jnp.float32)
    mem = jax.random.normal(ks[1], (BATCH, MEM_LEN, D_MODEL), jnp.float32)
    positions = (jax.random.randint(ks[2], (BATCH, 1), 0, 1024, dtype=jnp.int32)
                 + jnp.arange(SEQ, dtype=jnp.int32)[None, :])
    return {
        'x': x,
        'mem': mem,
        'positions': positions,
        'norm_mix': gain(ks[3], (DEPTH, D_MODEL)),
        'norm_xattn': gain(ks[4], (DEPTH, D_MODEL)),
        'norm_ffn': gain(ks[5], (DEPTH, D_MODEL)),
        'norm_memory': gain(ks[6], (D_MODEL,)),
        'norm_final': gain(ks[7], (D_MODEL,)),
        'pool_w': nrm(ks[8], (N_POOL_LAYERS, POOL_GROUPS, POOL_GC, POOL_GC), POOL_GC),
        'pool_scale': 1.0 + 0.1 * jax.random.normal(ks[9], (N_POOL_LAYERS, D_MODEL), jnp.float32),
        'attn_w_in': nrm(ks[10], (N_ATTN_LAYERS, D_MODEL, ATTN_IN), D_MODEL),
        'attn_w_out': nrm(ks[11], (N_ATTN_LAYERS, N_HEADS * HEAD_DIM, D_MODEL), N_HEADS * HEAD_DIM),
        'xattn_w_q': nrm(ks[12], (DEPTH, D_MODEL, X_HEADS * X_HEAD_DIM), D_MODEL),
        'xattn_w_kv': nrm(ks[13], (DEPTH, D_MODEL, 2 * X_HEADS * X_HEAD_DIM), D_MODEL),
        'xattn_w_o': nrm(ks[14], (DEPTH, X_HEADS * X_HEAD_DIM, D_MODEL), X_HEADS * X_HEAD_DIM),
        'ffn_w_in': nrm(ks[15], (DEPTH, D_MODEL, 2 * FFN_HIDDEN), D_MODEL),
        'ffn_w_out': nrm(ks[16], (DEPTH, FFN_HIDDEN, D_MODEL), FFN_HIDDEN),
    }


def reference(x, mem, positions, norm_mix, norm_xattn, norm_ffn, norm_memory, norm_final,
              pool_w, pool_scale, attn_w_in, attn_w_out, xattn_w_q, xattn_w_kv, xattn_w_o,
              ffn_w_in, ffn_w_out):
    cos_a, sin_a = rope_tables(positions, HEAD_DIM)
    cos_i, sin_i = rope_tables(positions, IDX_DIM)
    memn = rms_norm(mem, norm_memory)
    ia = 0
    ib = 0
    for i in range(DEPTH):
        h = rms_norm(x, norm_mix[i])
        if i % 2 == 0:
            x = x + pool_mixer(h, pool_w[ia], pool_scale[ia])
            ia += 1
        else:
            x = x + dsa_attention(h, attn_w_in[ib], attn_w_out[ib], cos_a, sin_a, cos_i, sin_i)
            ib += 1
        x = x + memory_cross_attention(rms_norm(x, norm_xattn[i]), memn,
                                       xattn_w_q[i], xattn_w_kv[i], xattn_w_o[i])
        x = x + swiglu(rms_norm(x, norm_ffn[i]), ffn_w_in[i], ffn_w_out[i])
    return rms_norm(x, norm_final)
```

```python
import math
from contextlib import ExitStack
import numpy as np
import concourse.bass as bass
import concourse.mybir as mybir
from concourse.bass_utils import run_bass_kernel_spmd

F32 = mybir.dt.float32
BF16 = mybir.dt.bfloat16
I32 = mybir.dt.int32
ALU = mybir.AluOpType
AF = mybir.ActivationFunctionType

D = 2048
S = 2048
T = 512
NTS = S // T
NC = 16
DEPTH = 4
HID = 5632
NHC = HID // 128
MEM = 256
EPS = 1e-6
PI = math.pi
TWO_PI = 2.0 * math.pi
WINS = (2, 4, 8, 16)
NBIS = 16
TOPK = 256

G_MIX, G_XAT, G_FFN, G_MEM, G_FIN, G_PSC = 0, 4, 8, 12, 13, 14
NGV = 16


class Res:
    __slots__ = ("name", "w", "r", "dsem", "dcnt")

    def __init__(self, name):
        self.name = name
        self.w = None
        self.r = {}
        self.dsem = None
        self.dcnt = 0


def _merge(dst, ev):
    k = id(ev[0])
    old = dst.get(k)
    if old is None or old[1] < ev[1]:
        dst[k] = ev


class Prog:
    ENG = ("pe", "act", "dve", "pool", "sp")

    def __init__(self, nc, es):
        self.nc = nc
        self.es = es
        self.q = {e: [] for e in self.ENG}
        self.sem = {e: es.enter_context(nc.semaphore("s_" + e)) for e in self.ENG}
        self.cnt = {e: 0 for e in self.ENG}
        self.waited = {e: {} for e in self.ENG}
        self.pend = {e: ([], []) for e in self.ENG}
        self.nsem = 0
        self.dsems = {}
        self.finals = []

    def _wait(self, eng, ev):
        sem, val = ev
        if eng == "pe" and sem is self.sem["pe"]:
            return
        k = id(sem)
        if self.waited[eng].get(k, 0) >= val:
            return
        self.waited[eng][k] = val
        self.q[eng].append(("wait", sem, val))

    def _deps(self, eng, reads, writes):
        for r in reads:
            if r.w is not None:
                self._wait(eng, r.w)
        for w in writes:
            if w.w is not None:
                self._wait(eng, w.w)
            for ev in list(w.r.values()):
                self._wait(eng, ev)

    def op(self, eng, fn, reads=(), writes=(), signal=True):
        self._deps(eng, reads, writes)
        pr, pw = self.pend[eng]
        pr.extend(reads)
        pw.extend(writes)
        if signal:
            self.cnt[eng] += 1
            ev = (self.sem[eng], self.cnt[eng])
            for r in pr:
                _merge(r.r, ev)
            for w in pw:
                w.w = ev
                w.r = {}
            self.pend[eng] = ([], [])
        self.q[eng].append(("op", fn, signal))

    def dma(self, queue, pairs, reads=(), writes=(), owner=None, final=False):
        self._deps(queue, reads, writes)
        if owner.dsem is None:
            if owner.name not in self.dsems:
                self.dsems[owner.name] = [self.es.enter_context(self.nc.semaphore("d%d" % self.nsem)), 0]
                self.nsem += 1
            owner.dsem = self.dsems[owner.name]
        owner.dsem[1] += 16 * len(pairs)
        ev = (owner.dsem[0], owner.dsem[1])
        for r in reads:
            _merge(r.r, ev)
        for w in writes:
            w.w = ev
            w.r = {}
        if final:
            self.finals.append(ev)
        self.q[queue].append(("dma", pairs, owner.dsem[0]))

    def finish(self):
        for e in self.ENG:
            assert not self.pend[e][0] and not self.pend[e][1], e
        for ev in self.finals:
            self._wait("sp", ev)
        for e in ("pe", "act", "dve"):
            if self.cnt[e]:
                self._wait("sp", (self.sem[e], self.cnt[e]))

    def emit(self):
        def runner(name):
            def f(e):
                for item in self.q[name]:
                    if item[0] == "wait":
                        e.wait_ge(item[1], item[2])
                    elif item[0] == "op":
                        ins = item[1](e)
                        if item[2]:
                            ins.then_inc(self.sem[name], 1)
                    else:
                        for (o, i) in item[1]:
                            e.dma_start(out=o, in_=i).then_inc(item[2], 16)
            return f

        with self.nc.Block() as block:
            block.tensor(runner("pe"))
            block.scalar(runner("act"))
            block.vector(runner("dve"))
            block.gpsimd(runner("pool"))
            block.sync(runner("sp"))


class Buf:
    def __init__(self, ap, res):
        self.ap = ap
        self.res = res if isinstance(res, list) else [res]

    def __getitem__(self, k):
        return self.ap[k]


def host_consts():
    c = {}
    c["ident"] = np.eye(128, dtype=np.float32)
    c["ones"] = np.ones((128, 128), np.float32)
    m = np.arange(128)
    pa = np.zeros((128, 128), np.float32)
    pa[(m + 64) % 128, m] = 1.0
    c["permA"] = pa
    pi_ = np.zeros((128, 128), np.float32)
    pi_[(m // 64) * 64 + ((m % 64) + 32) % 64, m] = 1.0
    c["permI"] = pi_
    s_ = np.arange(128)[:, None]
    t_ = np.arange(128)[None, :]
    c["caus01"] = (s_ <= t_).astype(np.float32)
    c["causadd"] = np.where(t_ <= s_, 0.0, -1e30).astype(np.float32)
    invA = (10000.0 ** (-(np.arange(0, 128, 2, dtype=np.float32) / 128))).astype(np.float32)
    invI = (10000.0 ** (-(np.arange(0, 64, 2, dtype=np.float32) / 64))).astype(np.float32)
    sm = np.zeros((128, 8), np.float32)
    sm[:, 0] = invA[m % 64]
    sm[:, 1] = np.where(m < 64, -1.0, 1.0)
    sm[:, 2] = invI[m % 32]
    sm[:, 3] = np.where((m % 64) < 32, -1.0, 1.0)
    c["small"] = sm
    c["pw"] = np.tile((2.0 ** -(np.arange(NBIS) + 1.0)).astype(np.float32)[None, :], (128, 1))
    fix = np.zeros((128, 4, 16), np.float32)
    for g, w in enumerate(WINS):
        for t in range(16):
            fix[:, g, t] = w / min(t + 1, w)
    c["fix"] = fix.reshape(128, 64)
    return c


CST_LAYOUT = [("ident", 128), ("ones", 128), ("permA", 128), ("permI", 128), ("caus01", 128),
              ("causadd", 128), ("small", 8), ("pw", NBIS), ("fix", 64)]
CST_OFF = {}
_o = 0
for _n, _w in CST_LAYOUT:
    CST_OFF[_n] = (_o, _w)
    _o += _w
NCST = _o


def pack_consts():
    c = host_consts()
    out = np.zeros((128, NCST), np.float32)
    for n, w in CST_LAYOUT:
        o, _ = CST_OFF[n]
        out[:, o:o + w] = c[n]
    return out


def build(cfg):
    nseq = cfg.get("nseq", 2)
    ntile = cfg.get("ntile", NTS)
    nlayer = cfg.get("nlayer", DEPTH)
    stop = cfg.get("stop", None)
    nc = bass.Bass("TRN2", target_bir_lowering=False)
    es = ExitStack()
    with es:
        _build(nc, es, nseq, ntile, nlayer, stop, cfg.get("dbg", 9), cfg.get("dbgp", 9))
    return nc


def _build(nc, es, nseq, ntile, nlayer, stop, dbg=9, dbgp=9):
    P = Prog(nc, es)

    def dram(name, shape, dt=F32, kind="ExternalInput"):
        return nc.dram_tensor(name, list(shape), dt, kind=kind).ap()

    x_d = dram("x", [2, S, D])
    mem_d = dram("mem", [2, MEM, D])
    pos_d = dram("pos", [2, 128, S], I32)
    gains_d = dram("gains", [128, NGV * 16])
    cst_d = dram("cst", [128, NCST])
    WSHAPES = {"pool_w": [2, 4, 512, 512], "attn_w_in": [2, D, 3584], "attn_w_out": [2, D, D],
               "xattn_w_q": [4, D, 512], "xattn_w_kv": [4, D, 1024], "xattn_w_o": [4, 512, D],
               "ffn_w_in": [4, D, 2 * HID], "ffn_w_out": [4, HID, D], "kiwi": [2, D, 256]}
    wd_cache = {}

    def wd(name, l):
        key = "%s_%d" % (name, l)
        if key not in wd_cache:
            wd_cache[key] = dram(key, WSHAPES[name][1:])
        return wd_cache[key]

    out_d = dram("out", [2, S, D], kind="ExternalOutput")

    def sb(name, shape, dt):
        return es.enter_context(nc.sbuf_tensor("sb_" + name, list(shape), dt))

    xT_t = sb("xT", [128, NC, T], F32)
    xT_res = [Res("xT%d" % c) for c in range(NC)]
    hT_t = sb("hT", [128, NC, T], BF16)
    hT_res = [Res("hT%d" % c) for c in range(NC)]
    gains_t = sb("gains", [128, NGV * 16], F32)
    gains = Buf(gains_t, Res("gains"))
    cstf_t = sb("cstf", [128, NCST], F32)
    cstf = Buf(cstf_t, Res("cstf"))
    cstb_t = sb("cstb", [128, 5 * 128], BF16)
    cstb = Buf(cstb_t, Res("cstb"))
    eps_t = sb("eps", [128, 4], F32)
    epsb = Buf(eps_t, Res("eps"))

    def cf(name):
        o, w = CST_OFF[name]
        return cstf_t[:, o:o + w]

    def cb(name):
        o, w = CST_OFF[name]
        return cstb_t[:, o:o + w]

    NSLOT = 5
    SLOT_E = 4096
    slots = []
    for i in range(NSLOT):
        t_ = sb("wslot%d" % i, [128, SLOT_E], BF16)
        slots.append(Buf(t_[:, :], Res("wslot%d" % i)))
    slot_i = [0]

    ARENA_B = 48 * 1024
    arena_t = sb("arena", [128, ARENA_B // 2], BF16)
    GR = 1024
    gran_owner = [None] * (ARENA_B // GR)

    def _claim(res, owners):
        seen = set()
        for ob in owners:
            if ob is None or id(ob) in seen:
                continue
            seen.add(id(ob))
            for rr in ob.res:
                if rr.w is not None:
                    _merge(res.r, rr.w)
                for ev in rr.r.values():
                    _merge(res.r, ev)

    def aview(name, off, shape, dt):
        esz = 2 if dt == BF16 else 4
        n = int(np.prod(shape[1:]))
        nb = n * esz
        assert off % 4 == 0 and off + nb <= ARENA_B, (name, off, nb)
        ap = arena_t[:, off // 2:(off + nb) // 2]
        if dt != BF16:
            ap = ap.bitcast(dt)
        if len(shape) == 3:
            ap = ap.rearrange("p (a b) -> p a b", a=shape[1])
        res = Res(name)
        g0, g1 = off // GR, (off + nb - 1) // GR
        _claim(res, gran_owner[g0:g1 + 1])
        b = Buf(ap, res)
        for g in range(g0, g1 + 1):
            gran_owner[g] = b
        return b

    def hview(name, off, shape, dt):
        esz = 2 if dt == BF16 else 4
        n = int(np.prod(shape[1:]))
        nb = n * esz
        flat = hT_t[:, :, :].rearrange("p a b -> p (a b)")
        ap = flat[:, off // 2:(off + nb) // 2]
        if dt != BF16:
            ap = ap.bitcast(dt)
        if len(shape) == 3:
            ap = ap.rearrange("p (a b) -> p a b", a=shape[1])
        c0, c1 = off // 1024, (off + nb - 1) // 1024
        return Buf(ap, hT_res[c0:c1 + 1])

    banks = []
    for i in range(8):
        t_ = es.enter_context(nc.psum_tensor("bank%d" % i, [128, 512], F32))
        banks.append(Buf(t_[:, :], Res("bank%d" % i)))
    ring = {"a": 0, "b": 0, "ab": 0}

    def bank(r="ab"):
        if r == "a":
            i = ring["a"] % 4
            ring["a"] += 1
        elif r == "b":
            i = 4 + ring["b"] % 4
            ring["b"] += 1
        else:
            i = ring["ab"] % 8
            ring["ab"] += 1
        return banks[i]

    def wload(pairs_fn):
        s = slots[slot_i[0] % NSLOT]
        slot_i[0] += 1
        P.dma("pool", pairs_fn(s.ap), writes=s.res, owner=s.res[0])
        return s

    P.dma("sp", [(gains_t[:, :], gains_d[:, :])], writes=gains.res, owner=gains.res[0])
    P.dma("sp", [(cstf_t[:, :], cst_d[:, :])], writes=cstf.res, owner=cstf.res[0])
    P.dma("pool", [(cstb_t[:, :], cst_d[:, 0:5 * 128])], writes=cstb.res, owner=cstb.res[0])
    P.op("dve", lambda e: e.memset(eps_t[:, :], EPS), writes=epsb.res)

    ident_f = cf("ident")
    ident_b = cb("ident")
    ones_b = cb("ones")

    evac_flip = [0]

    def evac(dst_ap, dst_res, ps, ps_ap=None, extra_reads=()):
        src = ps.ap if ps_ap is None else ps_ap
        evac_flip[0] ^= 1
        if evac_flip[0]:
            P.op("act", lambda e: e.activation(out=dst_ap, in_=src, func=AF.Copy),
                 reads=ps.res + list(extra_reads), writes=dst_res)
        else:
            P.op("dve", lambda e: e.tensor_copy(out=dst_ap, in_=src),
                 reads=ps.res + list(extra_reads), writes=dst_res)

    def load_transpose(rows_ap_fn, ntok, dstT_ap_fn, dst_res_fn, stg):
        for j in range(ntok // 128):
            sg = stg[j % len(stg)]
            P.dma("sp", [(sg.ap, rows_ap_fn(j))], writes=sg.res, owner=sg.res[0])
            for c0 in range(0, NC, 4):
                ps = bank()
                for k in range(4):
                    c = c0 + k
                    P.op("pe", lambda e, c=c, k=k, ps=ps, sg=sg: e.transpose(
                        out=ps.ap[:, k * 128:(k + 1) * 128], in_=sg.ap[:, c * 128:(c + 1) * 128],
                        identity=ident_f),
                        reads=sg.res + cstf.res, writes=ps.res, signal=(k == 3))
                evac(dstT_ap_fn(c0, j), dst_res_fn(c0),
                     ps, ps.ap[:, :].rearrange("p (a b) -> p a b", a=4))

    def rmsnorm(srcT, src_res, ntok, gcol, dst_fn, sq_bufs, rs_bufs):
        ps = bank()
        for q4 in range(4):
            sq = sq_bufs[q4 % len(sq_bufs)]
            sqv = sq.ap[:, :, 0:ntok]
            P.op("act", lambda e, q4=q4, sqv=sqv: e.activation(
                out=sqv, in_=srcT[:, q4 * 4:(q4 + 1) * 4, :], func=AF.Square),
                reads=src_res[q4 * 4:(q4 + 1) * 4], writes=sq.res)
            for k in range(4):
                c = q4 * 4 + k
                P.op("pe", lambda e, c=c, k=k, sqv=sqv: e.matmul(
                    ps.ap[:, 0:ntok], lhsT=ones_b, rhs=sqv[:, k, :], start=(c == 0), stop=(c == NC - 1)),
                    reads=sq.res + cstb.res, writes=ps.res, signal=(k == 3))
        t1, rstd = rs_bufs
        P.op("act", lambda e: e.activation(out=t1.ap[:, 0:ntok], in_=ps.ap[:, 0:ntok], func=AF.Sqrt,
                                           bias=eps_t[:, 0:1], scale=1.0 / D),
             reads=ps.res + epsb.res, writes=t1.res)
        P.op("dve", lambda e: e.reciprocal(out=rstd.ap[:, 0:ntok], in_=t1.ap[:, 0:ntok]),
             reads=t1.res, writes=rstd.res)
        for c in range(NC):
            o_ap, o_res = dst_fn(c)
            P.op("dve", lambda e, c=c, o_ap=o_ap: e.scalar_tensor_tensor(
                out=o_ap, in0=srcT[:, c, :], scalar=gains_t[:, gcol * 16 + c:gcol * 16 + c + 1],
                in1=rstd.ap[:, 0:ntok], op0=ALU.mult, op1=ALU.mult),
                reads=[src_res[c]] + rstd.res + gains.res, writes=o_res)

    t1_t = sb("t1", [128, T], F32)
    rstd_t = sb("rstd", [128, T], F32)
    rsb = [Buf(t1_t[:, :], Res("t1")), Buf(rstd_t[:, :], Res("rstd"))]
    xK_t = sb("xK", [128, DEPTH, 4, MEM], BF16)
    xV_t = sb("xV", [128, DEPTH, 2, 512], BF16)
    xK_res = [Res("xK%d" % l) for l in range(DEPTH)]
    xV_res = [Res("xV%d" % l) for l in range(DEPTH)]
    Kc_t = [sb("Kc%d" % i, [128, S], BF16) for i in range(2)]
    Vc_t = [sb("Vc%d" % i, [128, S // 128, 128], BF16) for i in range(2)]
    kic_t = [sb("kic%d" % i, [128, S], BF16) for i in range(2)]
    Kc_res = [[Res("Kc%d_%d" % (i, j)) for j in range(NTS)] for i in range(2)]
    Vc_res = [[Res("Vc%d_%d" % (i, j)) for j in range(NTS)] for i in range(2)]
    kic_res = [[Res("kic%d_%d" % (i, j)) for j in range(NTS)] for i in range(2)]
    halo_t = [sb("halo%d" % i, [128, NC, 16], F32) for i in range(2)]
    halo_res = [[Res("halo%d_%d" % (i, g)) for g in range(4)] for i in range(2)]
    rope_t = [sb("rope%d" % i, [128, T], F32) for i in range(4)]
    rope_res = [Res("rope%d" % i) for i in range(4)]
    wi_t = sb("wi", [128, 64], F32)
    wi_res = Res("wi")
    bis_t = sb("bis", [128, 64], F32)
    bis_res = Res("bis")
    posi_t = sb("posi", [128, T], I32)
    posi_res = Res("posi")

    def sq_bufs():
        return [aview("sq0", 40960, [128, 4, T], BF16), aview("sq1", 45056, [128, 4, T], BF16)]

    def add_resid(dc, ps, scale_ap=None):
        if scale_ap is None:
            P.op("dve", lambda e: e.tensor_tensor(out=xT_t[:, dc, :], in0=ps.ap, in1=xT_t[:, dc, :], op=ALU.add),
                 reads=ps.res, writes=[xT_res[dc]])
        else:
            P.op("dve", lambda e: e.scalar_tensor_tensor(out=xT_t[:, dc, :], in0=ps.ap, scalar=scale_ap,
                                                         in1=xT_t[:, dc, :], op0=ALU.mult, op1=ALU.add),
                 reads=ps.res + gains.res, writes=[xT_res[dc]])

    def slot3(s_, a, b):
        return s_.ap[:, 0:a * b].rearrange("p (a b) -> p a b", a=a)

    def mm_group(ps_ap, ps, lhs_fn, rhs_fn, n, reads):
        for k in range(n):
            la_, ra_ = lhs_fn(k), rhs_fn(k)
            P.op("pe", lambda e, k=k, la_=la_, ra_=ra_: e.matmul(ps_ap, lhsT=la_, rhs=ra_, start=(k == 0), stop=(k == n - 1)),
                 reads=reads, writes=ps.res, signal=(k == n - 1))

    def ffn_block(l):
        rmsnorm(xT_t, xT_res, T, G_FFN + l, lambda c: (hT_t[:, c, :], [hT_res[c]]), sq_bufs(), rsb)
        w_in = wd("ffn_w_in", l)
        w_out = wd("ffn_w_out", l)
        HH = NHC // 2
        for hh in range(2):
            aT = aview("aT", 0, [128, HH, T], BF16)
            sgb = [aview("sg0", 22528, [128, T], F32), aview("sg1", 24576, [128, T], F32)]
            for mp in range(HH // 2):
                hc0 = hh * HH + 2 * mp

                def pfg(ap, hc0=hc0):
                    v = ap[:, 0:4096].rearrange("p (c n) -> p c n", c=16)
                    return [(v, w_in[:, hc0 * 128:(hc0 + 2) * 128].rearrange("(c p) n -> p c n", p=128))]

                def pfu(ap, hc0=hc0):
                    v = ap[:, 0:4096].rearrange("p (c n) -> p c n", c=16)
                    return [(v, w_in[:, HID + hc0 * 128:HID + (hc0 + 2) * 128].rearrange("(c p) n -> p c n", p=128))]
                slg = wload(pfg)
                slu = wload(pfu)
                svg = slot3(slg, 16, 256)
                svu = slot3(slu, 16, 256)
                for m2 in range(2):
                    m = 2 * mp + m2
                    psg = bank()
                    mm_group(psg.ap, psg, lambda k: svg[:, k, m2 * 128:(m2 + 1) * 128], lambda k: hT_t[:, k, :], NC, slg.res + hT_res)
                    psu = bank()
                    mm_group(psu.ap, psu, lambda k: svu[:, k, m2 * 128:(m2 + 1) * 128], lambda k: hT_t[:, k, :], NC, slu.res + hT_res)
                    sg = sgb[m % 2]
                    P.op("act", lambda e, sg=sg, psg=psg: e.activation(out=sg.ap, in_=psg.ap, func=AF.Silu),
                         reads=psg.res, writes=sg.res)
                    P.op("dve", lambda e, sg=sg, psu=psu, m=m, aT=aT: e.tensor_tensor(out=aT.ap[:, m, :], in0=sg.ap, in1=psu.ap, op=ALU.mult),
                         reads=sg.res + psu.res, writes=aT.res)
            for dc in range(NC):
                def pf(ap, dc=dc, hh=hh):
                    v = ap[:, 0:HH * 128].rearrange("p (k n) -> p k n", k=HH)
                    return [(v, w_out[hh * HH * 128:(hh + 1) * HH * 128, dc * 128:(dc + 1) * 128].rearrange("(k p) n -> p k n", p=128))]
                sl = wload(pf)
                sv = slot3(sl, HH, 128)
                ps = bank()
                mm_group(ps.ap, ps, lambda k: sv[:, k, :], lambda k: aT.ap[:, k, :], HH, sl.res + aT.res)
                add_resid(dc, ps)

    def xattn_setup(s):
        memT = aview("memT", 0, [128, NC, MEM], F32)
        stg = [aview("mstg0", 16384, [128, D], F32), aview("mstg1", 24576, [128, D], F32)]
        load_transpose(lambda j: mem_d[s, j * 128:(j + 1) * 128, :], MEM,
                       lambda c0, j: memT.ap[:, c0:c0 + 4, j * 128:(j + 1) * 128],
                       lambda c0: memT.res, stg)
        memn = aview("memn", 32768, [128, NC, MEM], BF16)
        rmsnorm(memT.ap, memT.res * NC, MEM, G_MEM, lambda c: (memn.ap[:, c, :], memn.res), sq_bufs(), rsb)
        for l in range(nlayer):
            w = wd("xattn_w_kv", l)
            for pp in range(2):
                sl = wload(lambda ap, pp=pp: [(ap[:, 0:4096].rearrange("p (c n) -> p c n", c=16),
                                              w[:, pp * 256:(pp + 1) * 256].rearrange("(c p) n -> p c n", p=128))])
                sv = slot3(sl, 16, 256)
                for h2 in range(2):
                    hq = 2 * pp + h2
                    ps = bank()
                    mm_group(ps.ap[:, 0:MEM], ps, lambda k: sv[:, k, h2 * 128:(h2 + 1) * 128], lambda k: memn.ap[:, k, :],
                             NC, sl.res + memn.res)
                    evac(xK_t[:, l, hq, :], [xK_res[l]], ps, ps.ap[:, 0:MEM])
            for vh in range(2):
                sl = wload(lambda ap, vh=vh: [(ap[:, 0:4096].rearrange("p (c n) -> p c n", c=16),
                                              w[:, 512 + vh * 256:512 + (vh + 1) * 256].rearrange("(c p) n -> p c n", p=128))])
                sv = slot3(sl, 16, 256)
                for mc in range(2):
                    ps = bank()
                    mm_group(ps.ap[:, 0:256], ps, lambda k: memn.ap[:, k, mc * 128:(mc + 1) * 128], lambda k: sv[:, k, :],
                             NC, sl.res + memn.res)
                    evac(xV_t[:, l, mc, vh * 256:(vh + 1) * 256], [xV_res[l]], ps, ps.ap[:, 0:256])

    def xattn_block(l):
        rmsnorm(xT_t, xT_res, T, G_XAT + l, lambda c: (hT_t[:, c, :], [hT_res[c]]), sq_bufs(), rsb)
        qT = aview("xqT", 0, [128, 4, T], BF16)
        PTb = [aview("xPT0", 4096, [128, 2, T], BF16), aview("xPT1", 6144, [128, 2, T], BF16)]
        rdb = [aview("xrd0", 8192, [128, T], F32), aview("xrd1", 10240, [128, T], F32)]
        oT = aview("xoT", 12288, [128, 4, T], BF16)
        wq = wd("xattn_w_q", l)
        for pp in range(2):
            sl = wload(lambda ap, pp=pp: [(ap[:, 0:4096].rearrange("p (c n) -> p c n", c=16),
                                          wq[:, pp * 256:(pp + 1) * 256].rearrange("(c p) n -> p c n", p=128))])
            sv = slot3(sl, 16, 256)
            for h2 in range(2):
                hq = 2 * pp + h2
                ps = bank()
                mm_group(ps.ap, ps, lambda k: sv[:, k, h2 * 128:(h2 + 1) * 128], lambda k: hT_t[:, k, :], NC, sl.res + hT_res)
                evac(qT.ap[:, hq, :], qT.res, ps)
        sc = 128.0 ** -0.5
        for hq in range(4):
            PT = PTb[hq % 2]
            for mc in range(2):
                ps = bank()
                P.op("pe", lambda e, ps=ps, mc=mc, hq=hq: e.matmul(ps.ap, lhsT=xK_t[:, l, hq, mc * 128:(mc + 1) * 128],
                                                                  rhs=qT.ap[:, hq, :], start=True, stop=True),
                     reads=[xK_res[l]] + qT.res, writes=ps.res)
                P.op("act", lambda e, ps=ps, mc=mc, PT=PT: e.activation(out=PT.ap[:, mc, :], in_=ps.ap, func=AF.Exp, scale=sc),
                     reads=ps.res, writes=PT.res)
            psd = bank()
            mm_group(psd.ap, psd, lambda k: ones_b, lambda k: PT.ap[:, k, :], 2, PT.res + cstb.res)
            pso = bank()
            mm_group(pso.ap, pso, lambda k: xV_t[:, l, k, hq * 128:(hq + 1) * 128], lambda k: PT.ap[:, k, :], 2,
                     PT.res + [xV_res[l]])
            rd = rdb[hq % 2]
            P.op("dve", lambda e, rd=rd, psd=psd: e.reciprocal(out=rd.ap, in_=psd.ap), reads=psd.res, writes=rd.res)
            P.op("dve", lambda e, rd=rd, pso=pso, hq=hq: e.tensor_tensor(out=oT.ap[:, hq, :], in0=pso.ap, in1=rd.ap, op=ALU.mult),
                 reads=pso.res + rd.res, writes=oT.res)
        wo = wd("xattn_w_o", l)
        for ch in range(2):
            sl = wload(lambda ap, ch=ch: [(ap[:, 0:4096].rearrange("p (h n) -> p h n", h=4),
                                          wo[:, ch * 1024:(ch + 1) * 1024].rearrange("(h p) n -> p h n", p=128))])
            sv = slot3(sl, 4, 1024)
            for d8 in range(8):
                dc = ch * 8 + d8
                ps = bank()
                mm_group(ps.ap, ps, lambda k: sv[:, k, d8 * 128:(d8 + 1) * 128], lambda k: oT.ap[:, k, :], 4, sl.res + oT.res)
                add_resid(dc, ps)

    def pool_block(l, ti):
        ia = l // 2
        pA = [aview("pA%d" % g, g * 8448, [128, 4, 16 + T], F32) for g in range(4)]
        rmsnorm(xT_t, xT_res, T, G_MIX + l, lambda c: (pA[c // 4].ap[:, c % 4, 16:16 + T], pA[c // 4].res), sq_bufs(), rsb)
        W = 16 + T
        pw_d = wd("pool_w", ia)
        for g in range(4):
            A = pA[g]
            hres = [halo_res[ia][g]]
            if ti == 0:
                P.op("dve", lambda e, g=g: e.memset(halo_t[ia][:, 4 * g:4 * g + 4, :], 0.0), writes=hres)
            P.op("dve", lambda e, g=g, A=A: e.tensor_copy(out=A.ap[:, :, 0:16], in_=halo_t[ia][:, 4 * g:4 * g + 4, :]),
                 reads=hres, writes=A.res)
            P.op("dve", lambda e, g=g, A=A: e.tensor_copy(out=halo_t[ia][:, 4 * g:4 * g + 4, :], in_=A.ap[:, :, T:T + 16]),
                 reads=A.res, writes=hres)
            B = aview("pB", 33792, [128, 4, W], F32)
            C = hview("pC", 0, [128, 4, W], F32)

            def shadd(dst, src, sh, lo):
                P.op("dve", lambda e: e.tensor_tensor(out=dst.ap[:, :, lo:W], in0=src.ap[:, :, lo:W],
                                                      in1=src.ap[:, :, lo - sh:W - sh], op=ALU.add),
                     reads=src.res, writes=dst.res)
            shadd(B, A, 1, 1)
            ws = B
            if g >= 1:
                shadd(C, B, 2, 3)
                ws = C
            if g >= 2:
                shadd(B, C, 4, 7)
                ws = B
            if g >= 3:
                shadd(C, B, 8, 15)
                ws = C
            if ti == 0:
                fo = CST_OFF["fix"][0] + g * 16
                for k in range(4):
                    P.op("dve", lambda e, k=k, ws=ws, fo=fo: e.tensor_tensor(out=ws.ap[:, k, 16:32], in0=ws.ap[:, k, 16:32],
                                                                          in1=cstf_t[:, fo:fo + 16], op=ALU.mult),
                         reads=ws.res + cstf.res, writes=ws.res)
            if g % 2 == 0:
                df = aview("pd0", 42240, [128, 4, T], BF16)
            else:
                df = hview("pd1", 8448, [128, 4, T], BF16)
            P.op("dve", lambda e, ws=ws, A=A, df=df, g=g: e.scalar_tensor_tensor(
                out=df.ap, in0=ws.ap[:, :, 16:W], scalar=1.0 / WINS[g], in1=A.ap[:, :, 16:W], op0=ALU.mult, op1=ALU.subtract),
                reads=ws.res + A.res, writes=df.res)
            sl = wload(lambda ap, g=g: [(ap[:, 0:2048].rearrange("p (k n) -> p k n", k=4),
                                        pw_d[g].rearrange("(k p) n -> p k n", p=128))])
            sv = slot3(sl, 4, 512)
            for oc in range(4):
                ps = bank()
                mm_group(ps.ap, ps, lambda k: sv[:, k, oc * 128:(oc + 1) * 128], lambda k: df.ap[:, k, :], 4, sl.res + df.res)
                dc = 4 * g + oc
                add_resid(dc, ps, gains_t[:, (G_PSC + ia) * 16 + dc:(G_PSC + ia) * 16 + dc + 1])

    def rope_tables(s, t0):
        posf = aview("posf", 0, [128, T], F32)
        ang = aview("ang", 2048, [128, T], F32)
        ki_ = aview("kint", 4096, [128, T], F32)
        kf = aview("kf", 6144, [128, T], F32)
        r_ = aview("rr", 8192, [128, T], F32)
        m_ = aview("mm", 10240, [128, T], F32)
        P.dma("pool", [(posf.ap, pos_d[s, :, t0:t0 + T])], writes=posf.res, owner=posi_res)
        so = CST_OFF["small"][0]
        for fam in range(2):
            invf = cstf_t[:, so + 2 * fam:so + 2 * fam + 1]
            sgn = cstf_t[:, so + 2 * fam + 1:so + 2 * fam + 2]
            for which in range(2):
                dst = rope_t[2 * fam + which]
                dres = [rope_res[2 * fam + which]]
                P.op("dve", lambda e, invf=invf, which=which: e.tensor_scalar(
                    out=ang.ap, in0=posf.ap, scalar1=invf, scalar2=(PI / 2 if which == 0 else 0.0), op0=ALU.mult, op1=ALU.add),
                    reads=posf.res + cstf.res, writes=ang.res)
                P.op("dve", lambda e: e.tensor_scalar(out=ki_.ap, in0=ang.ap, scalar1=1.0 / TWO_PI, scalar2=12582912.0,
                                                      op0=ALU.mult, op1=ALU.add), reads=ang.res, writes=ki_.res)
                P.op("dve", lambda e: e.tensor_scalar(out=kf.ap, in0=ki_.ap, scalar1=-12582912.0, scalar2=None, op0=ALU.add),
                     reads=ki_.res, writes=kf.res)
                P.op("dve", lambda e: e.scalar_tensor_tensor(out=r_.ap, in0=kf.ap, scalar=-TWO_PI, in1=ang.ap, op0=ALU.mult, op1=ALU.add),
                     reads=kf.res + ang.res, writes=r_.res)
                P.op("dve", lambda e: e.tensor_scalar(out=m_.ap, in0=r_.ap, scalar1=PI, scalar2=None, op0=ALU.is_gt),
                     reads=r_.res, writes=m_.res)
                P.op("dve", lambda e: e.scalar_tensor_tensor(out=ang.ap, in0=m_.ap, scalar=-TWO_PI, in1=r_.ap, op0=ALU.mult, op1=ALU.add),
                     reads=m_.res + r_.res, writes=ang.res)
                P.op("dve", lambda e: e.tensor_scalar(out=m_.ap, in0=ang.ap, scalar1=-PI, scalar2=None, op0=ALU.is_lt),
                     reads=ang.res, writes=m_.res)
                P.op("dve", lambda e: e.scalar_tensor_tensor(out=r_.ap, in0=m_.ap, scalar=TWO_PI, in1=ang.ap, op0=ALU.mult, op1=ALU.add),
                     reads=m_.res + ang.res, writes=r_.res)
                P.op("dve", lambda e: e.tensor_scalar(out=r_.ap, in0=r_.ap, scalar1=PI, scalar2=-PI, op0=ALU.min, op1=ALU.max),
                     reads=r_.res, writes=r_.res)
                SINF = AF.Copy if dbgp == 41 else AF.Sin
                if which == 0:
                    P.op("act", lambda e, dst=dst: e.activation(out=dst[:, :], in_=r_.ap, func=SINF), reads=r_.res, writes=dres)
                else:
                    P.op("act", lambda e: e.activation(out=kf.ap, in_=r_.ap, func=SINF), reads=r_.res, writes=kf.res)
                    P.op("dve", lambda e, dst=dst, sgn=sgn: e.tensor_scalar(out=dst[:, :], in0=kf.ap, scalar1=sgn, scalar2=None, op0=ALU.mult),
                         reads=kf.res + cstf.res, writes=dres)

    def dsa_block(l, ti):
        la = l // 2
        rmsnorm(xT_t, xT_res, T, G_MIX + l, lambda c: (hT_t[:, c, :], [hT_res[c]]), sq_bufs(), rsb)
        if dbg < -1:
            return
        w_in = wd("attn_w_in", la)
        Qr = aview("Qr", 0, [128, 16, T], BF16)
        Qr_res = [[Res("Qr%d_%d" % (hg, qt)) for qt in range(4)] for hg in range(4)]
        for hg in range(4):
            for qt in range(4):
                Qr_res[hg][qt].r = dict(Qr.res[0].r)
        Qr.res = [Qr_res[hg][qt] for hg in range(4) for qt in range(4)]
        qir = aview("qir", 16384, [128, 8, T], BF16)
        qsb = [aview("qsb0", 24576, [128, T], BF16), aview("qsb1", 25600, [128, T], BF16)]
        ra = aview("ra", 26624, [128, T], F32)
        rb = aview("rb", 28672, [128, T], F32)
        rflip = [0]

        def rope_apply(ps, fam, dst_ap, dst_res):
            if dbgp in (11, 31):
                evac(dst_ap, dst_res, ps)
                return
            cos_t, sin_t = rope_t[2 * fam], rope_t[2 * fam + 1]
            cres, sres = [rope_res[2 * fam]], [rope_res[2 * fam + 1]]
            perm = cb("permA") if fam == 0 else cb("permI")
            q = qsb[rflip[0] % 2]
            rflip[0] += 1
            P.op("act", lambda e: e.activation(out=q.ap, in_=ps.ap, func=AF.Copy), reads=ps.res, writes=q.res)
            P.op("dve", lambda e: e.tensor_tensor(out=ra.ap, in0=ps.ap, in1=cos_t[:, :], op=ALU.mult),
                 reads=ps.res + cres + q.res, writes=ra.res)
            ps2 = bank()
            P.op("pe", lambda e: e.matmul(ps2.ap, lhsT=perm, rhs=q.ap, start=True, stop=True),
                 reads=q.res + cstb.res, writes=ps2.res)
            P.op("dve", lambda e: e.tensor_tensor(out=rb.ap, in0=ps2.ap, in1=sin_t[:, :], op=ALU.mult),
                 reads=ps2.res + sres, writes=rb.res)
            P.op("dve", lambda e: e.tensor_tensor(out=dst_ap, in0=ra.ap, in1=rb.ap, op=ALU.add),
                 reads=ra.res + rb.res, writes=dst_res)

        def wcols(c0, n=256):
            return w_in[:, c0:c0 + n].rearrange("(c p) n -> p c n", p=128)

        for pp in range(8 if dbgp >= 1 else 0):
            if dbgp == 41 and pp >= 2:
                break
            if dbgp in (21, 31) and pp >= 2:
                break
            sl = wload(lambda ap, pp=pp: [(ap[:, 0:4096].rearrange("p (c n) -> p c n", c=16), wcols(pp * 256))])
            sv = slot3(sl, 16, 256)
            for h2 in range(2):
                h = 2 * pp + h2
                ps = bank()
                mm_group(ps.ap, ps, lambda k: sv[:, k, h2 * 128:(h2 + 1) * 128], lambda k: hT_t[:, k, :], NC, sl.res + hT_res)
                rope_apply(ps, 0, Qr.ap[:, h, :], Qr_res[h // 4])
        if dbgp < 2 or dbgp in (31, 41):
            return
        sl = wload(lambda ap: [(ap[:, 0:4096].rearrange("p (c n) -> p c n", c=16), wcols(2048))])
        sv = slot3(sl, 16, 256)
        ps = bank()
        mm_group(ps.ap, ps, lambda k: sv[:, k, 0:128], lambda k: hT_t[:, k, :], NC, sl.res + hT_res)
        rope_apply(ps, 0, Kc_t[la][:, ti * T:(ti + 1) * T], [Kc_res[la][ti]])
        for tq in range(4):
            ps = bank()
            mm_group(ps.ap[:, 0:128], ps, lambda k: hT_t[:, k, tq * 128:(tq + 1) * 128], lambda k: sv[:, k, 128:256],
                     NC, sl.res + hT_res)
            evac(Vc_t[la][:, ti * 4 + tq, :], [Vc_res[la][ti]], ps, ps.ap[:, 0:128])
        if dbgp < 3:
            return
        for pp in range(4):
            sl = wload(lambda ap, pp=pp: [(ap[:, 0:4096].rearrange("p (c n) -> p c n", c=16), wcols(2304 + pp * 256))])
            sv = slot3(sl, 16, 256)
            for h2 in range(2):
                j = 2 * pp + h2
                ps = bank()
                mm_group(ps.ap, ps, lambda k: sv[:, k, h2 * 128:(h2 + 1) * 128], lambda k: hT_t[:, k, :], NC, sl.res + hT_res)
                rope_apply(ps, 1, qir.ap[:, j, :], qir.res)
        if dbgp < 4:
            return
        kiwi_d = wd("kiwi", la)

        def pf(ap):
            v = ap[:, 0:16 * 256].rearrange("p (c n) -> p c n", c=16)
            return [(v, kiwi_d[:, :].rearrange("(c p) n -> p c n", p=128))]
        sl = wload(pf)
        sv = slot3(sl, 16, 256)
        ps = bank()
        mm_group(ps.ap, ps, lambda k: sv[:, k, 0:128], lambda k: hT_t[:, k, :], NC, sl.res + hT_res)
        rope_apply(ps, 1, kic_t[la][:, ti * T:(ti + 1) * T], [kic_res[la][ti]])
        for tq in range(4):
            ps = bank()
            mm_group(ps.ap[:, 0:128], ps, lambda k: hT_t[:, k, tq * 128:(tq + 1) * 128], lambda k: sv[:, k, 128:256],
                     NC, sl.res + hT_res)
            evac(wi_t[:, tq * 16:(tq + 1) * 16], [wi_res], ps, ps.ap[:, 0:16])

        if dbg < 1:
            return
        isc = hview("isc", 0, [128, S], F32)
        Mb = hview("Mb", 8192, [128, S], BF16)
        MT = hview("MT", 12288, [128, 16, 128], BF16)
        PTb = [aview("PT0", 30720, [128, 1024], BF16), aview("PT1", 32768, [128, 1024], BF16)]
        rlb = [aview("rl0", 34816, [128, 512], F32), aview("rl1", 36864, [128, 512], F32)]
        rden = aview("rden", 38912, [128, 1024], F32)
        sc = 128.0 ** -0.5
        co = CST_OFF["causadd"][0]
        pwo = CST_OFF["pw"][0]
        nflip = [0]
        for qt in range(4):
            j = ti * 4 + qt
            nkc = j + 1
            Lk = nkc * 128
            kres_all = [r for tt in range(ti + 1) for r in (kic_res[la][tt],)]
            Kres_all = [Kc_res[la][tt] for tt in range(ti + 1)]
            Vres_all = [Vc_res[la][tt] for tt in range(ti + 1)]
            qs = slice(qt * 128, (qt + 1) * 128)
            if j >= 2:
                nkb = (Lk + 511) // 512
                for kb in range(nkb):
                    k0 = kb * 512
                    kw = min(512, Lk - k0)
                    for h in range(16):
                        ch, base = h // 2, 64 * (h % 2)
                        ps = bank("a")
                        P.op("pe", lambda e, ps=ps, ch=ch, base=base, k0=k0, kw=kw, qs=qs: e.matmul(
                            ps.ap[:, 0:kw], lhsT=qir.ap[base:base + 64, ch, qs], rhs=kic_t[la][base:base + 64, k0:k0 + kw],
                            start=True, stop=True), reads=qir.res + kres_all, writes=ps.res)
                        rl = rlb[nflip[0] % 2]
                        nflip[0] += 1
                        P.op("act", lambda e, ps=ps, rl=rl, kw=kw: e.activation(out=rl.ap[:, 0:kw], in_=ps.ap[:, 0:kw], func=AF.Relu),
                             reads=ps.res, writes=rl.res)
                        wcol = wi_t[:, qt * 16 + h:qt * 16 + h + 1]
                        if h == 0:
                            P.op("dve", lambda e, rl=rl, k0=k0, kw=kw, wcol=wcol: e.tensor_scalar(
                                out=isc.ap[:, k0:k0 + kw], in0=rl.ap[:, 0:kw], scalar1=wcol, scalar2=None, op0=ALU.mult),
                                reads=rl.res + [wi_res], writes=isc.res)
                        else:
                            P.op("dve", lambda e, rl=rl, k0=k0, kw=kw, wcol=wcol: e.scalar_tensor_tensor(
                                out=isc.ap[:, k0:k0 + kw], in0=rl.ap[:, 0:kw], scalar=wcol, in1=isc.ap[:, k0:k0 + kw],
                                op0=ALU.mult, op1=ALU.add), reads=rl.res + [wi_res], writes=isc.res)
                X = mybir.AxisListType.X
                P.op("dve", lambda e, Lk=Lk: e.tensor_reduce(out=bis_t[:, 0:1], in_=isc.ap[:, 0:Lk], axis=X, op=ALU.min),
                     reads=isc.res, writes=[bis_res])
                P.op("dve", lambda e, j=j: e.tensor_tensor(out=isc.ap[:, j * 128:(j + 1) * 128], in0=isc.ap[:, j * 128:(j + 1) * 128],
                                                         in1=cstf_t[:, co:co + 128], op=ALU.add),
                     reads=isc.res + cstf.res, writes=isc.res)
                P.op("dve", lambda e, Lk=Lk: e.tensor_reduce(out=bis_t[:, 1:2], in_=isc.ap[:, 0:Lk], axis=X, op=ALU.max),
                     reads=isc.res, writes=[bis_res])
                P.op("dve", lambda e: e.tensor_tensor(out=bis_t[:, 1:2], in0=bis_t[:, 1:2], in1=bis_t[:, 0:1], op=ALU.subtract),
                     reads=[bis_res], writes=[bis_res])
                P.op("dve", lambda e: e.tensor_scalar(out=bis_t[:, 16:16 + NBIS], in0=cstf_t[:, pwo:pwo + NBIS],
                                                      scalar1=bis_t[:, 1:2], scalar2=None, op0=ALU.mult),
                     reads=[bis_res] + cstf.res, writes=[bis_res])
                for k in range(NBIS):
                    P.op("dve", lambda e, k=k: e.tensor_tensor(out=bis_t[:, 2:3], in0=bis_t[:, 0:1], in1=bis_t[:, 16 + k:17 + k], op=ALU.add),
                         reads=[bis_res], writes=[bis_res])
                    P.op("dve", lambda e, Lk=Lk: e.tensor_scalar(out=Mb.ap[:, 0:Lk], in0=isc.ap[:, 0:Lk], scalar1=bis_t[:, 2:3],
                                                               scalar2=None, op0=ALU.is_ge, op1=ALU.add, accum_out=bis_t[:, 3:4]),
                         reads=isc.res + [bis_res], writes=Mb.res + [bis_res])
                    P.op("dve", lambda e, k=k: e.tensor_scalar(out=bis_t[:, 4:5], in0=bis_t[:, 3:4], scalar1=TOPK - 0.5,
                                                             scalar2=bis_t[:, 16 + k:17 + k], op0=ALU.is_ge, op1=ALU.mult),
                         reads=[bis_res], writes=[bis_res])
                    P.op("dve", lambda e: e.tensor_tensor(out=bis_t[:, 0:1], in0=bis_t[:, 0:1], in1=bis_t[:, 4:5], op=ALU.add),
                         reads=[bis_res], writes=[bis_res])
                P.op("dve", lambda e, Lk=Lk: e.tensor_scalar(out=Mb.ap[:, 0:Lk], in0=isc.ap[:, 0:Lk], scalar1=bis_t[:, 0:1],
                                                           scalar2=None, op0=ALU.is_ge),
                     reads=isc.res + [bis_res], writes=Mb.res)
                for k0 in range(0, nkc, 4):
                    kn = min(4, nkc - k0)
                    ps = bank("a")
                    psb = ps.ap.bitcast(BF16)
                    for k in range(kn):
                        P.op("pe", lambda e, k=k, k0=k0, psb=psb: e.transpose(
                            out=psb[:, k * 128:(k + 1) * 128], in_=Mb.ap[:, (k0 + k) * 128:(k0 + k + 1) * 128], identity=ident_b),
                            reads=Mb.res + cstb.res, writes=ps.res, signal=(k == kn - 1))
                    evac(MT.ap[:, k0:k0 + kn, :], MT.res, ps, psb[:, 0:kn * 128].rearrange("p (a b) -> p a b", a=kn))
            else:
                for kc in range(nkc):
                    src = ones_b if kc < j else cb("caus01")
                    P.op("dve", lambda e, kc=kc, src=src: e.tensor_copy(out=MT.ap[:, kc, :], in_=src), reads=cstb.res, writes=MT.res)
            for half in range(2 if dbg >= 2 else 0):
                psO = [bank("b"), bank("b")]
                psD = [bank("b"), bank("b")]
                qres = [Qr_res[2 * half][qt], Qr_res[2 * half + 1][qt]]
                for kc in range(nkc):
                    psS = [bank("a"), bank("a")]
                    PT = PTb[nflip[0] % 2]
                    nflip[0] += 1
                    for i2 in range(2):
                        h0 = 8 * half + 4 * i2
                        P.op("pe", lambda e, i2=i2, h0=h0, kc=kc, psS=psS, qs=qs: e.matmul(
                            psS[i2].ap, lhsT=Kc_t[la][:, kc * 128:(kc + 1) * 128], rhs=Qr.ap[:, h0:h0 + 4, qs], start=True, stop=True),
                            reads=Kres_all + [qres[i2]], writes=psS[i2].res)
                        P.op("act", lambda e, i2=i2, psS=psS, PT=PT: e.activation(
                            out=PT.ap[:, i2 * 512:(i2 + 1) * 512], in_=psS[i2].ap, func=AF.Exp, scale=sc),
                            reads=psS[i2].res, writes=PT.res, signal=(i2 == 1))
                    pt3 = PT.ap.rearrange("p (h q) -> p h q", h=8)
                    P.op("dve", lambda e, pt3=pt3, kc=kc: e.tensor_tensor(
                        out=pt3, in0=pt3, in1=MT.ap[:, kc:kc + 1, :].to_broadcast([128, 8, 128]), op=ALU.mult),
                        reads=PT.res + MT.res, writes=PT.res)
                    for i2 in range(2):
                        P.op("pe", lambda e, i2=i2, kc=kc, PT=PT, psO=psO, nkc=nkc: e.matmul(
                            psO[i2].ap, lhsT=Vc_t[la][:, kc, :], rhs=PT.ap[:, i2 * 512:(i2 + 1) * 512],
                            start=(kc == 0), stop=(kc == nkc - 1)), reads=Vres_all + PT.res, writes=psO[i2].res,
                            signal=(kc == nkc - 1))
                        P.op("pe", lambda e, i2=i2, kc=kc, PT=PT, psD=psD, nkc=nkc: e.matmul(
                            psD[i2].ap, lhsT=ones_b, rhs=PT.ap[:, i2 * 512:(i2 + 1) * 512],
                            start=(kc == 0), stop=(kc == nkc - 1)), reads=cstb.res + PT.res, writes=psD[i2].res,
                            signal=True)
                for i2 in range(2):
                    h0 = 8 * half + 4 * i2
                    P.op("dve", lambda e, i2=i2, psD=psD: e.reciprocal(out=rden.ap[:, i2 * 512:(i2 + 1) * 512], in_=psD[i2].ap),
                         reads=psD[i2].res, writes=rden.res)
                    P.op("dve", lambda e, i2=i2, h0=h0, psO=psO, qs=qs: e.tensor_tensor(
                        out=Qr.ap[:, h0:h0 + 4, qs], in0=psO[i2].ap.rearrange("p (h q) -> p h q", h=4),
                        in1=rden.ap[:, i2 * 512:(i2 + 1) * 512].rearrange("p (h q) -> p h q", h=4), op=ALU.mult),
                        reads=psO[i2].res + rden.res, writes=[qres[i2]])
        if dbg < 3:
            return
        w_o = wd("attn_w_out", la)
        allq = [Qr_res[hg][qt] for hg in range(4) for qt in range(4)]
        for dp in range(8):
            sl = wload(lambda ap, dp=dp: [(ap[:, 0:4096].rearrange("p (h n) -> p h n", h=16),
                                          w_o[:, dp * 256:(dp + 1) * 256].rearrange("(h p) n -> p h n", p=128))])
            sv = slot3(sl, 16, 256)
            for d2 in range(2):
                dc = 2 * dp + d2
                ps = bank()
                mm_group(ps.ap, ps, lambda k: sv[:, k, d2 * 128:(d2 + 1) * 128], lambda k: Qr.ap[:, k, :], 16, sl.res + allq)
                add_resid(dc, ps)

    has_attn = any(l % 2 == 1 for l in range(nlayer))
    for s in range(nseq):
        if nlayer > 0:
            xattn_setup(s)
        for ti in range(ntile):
            t0 = ti * T
            stg = [aview("stg0", 0, [128, D], F32), aview("stg1", 8192, [128, D], F32)]
            load_transpose(lambda j: x_d[s, t0 + j * 128:t0 + (j + 1) * 128, :], T,
                           lambda c0, j: xT_t[:, c0:c0 + 4, j * 128:(j + 1) * 128],
                           lambda c0: xT_res[c0:c0 + 4], stg)
            if has_attn and dbgp != 31:
                rope_tables(s, t0)
            done = False
            for l in range(nlayer):
                if dbgp in (21, 31) and l == 0:
                    continue
                if l % 2 == 0:
                    pool_block(l, ti)
                else:
                    dsa_block(l, ti)
                if stop == "mix%d" % l:
                    done = True
                    break
                xattn_block(l)
                if stop == "xat%d" % l:
                    done = True
                    break
                ffn_block(l)
                if stop == "ffn%d" % l:
                    done = True
                    break
            if stop is None:
                rmsnorm(xT_t, xT_res, T, G_FIN, lambda c: (xT_t[:, c, :], [xT_res[c]]), sq_bufs(), rsb)
            ostg = [aview("ostg0", 0, [128, D], F32), aview("ostg1", 8192, [128, D], F32)]
            for j in range(T // 128):
                og = ostg[j % 2]
                for c0 in range(0, NC, 4):
                    ps = bank()
                    for k in range(4):
                        c = c0 + k
                        P.op("pe", lambda e, c=c, k=k, ps=ps, j=j: e.transpose(
                            out=ps.ap[:, k * 128:(k + 1) * 128], in_=xT_t[:, c, j * 128:(j + 1) * 128],
                            identity=ident_f),
                            reads=[xT_res[c]] + cstf.res, writes=ps.res, signal=(k == 3))
                    evac(og.ap[:, c0 * 128:(c0 + 4) * 128], og.res, ps)
                P.dma("sp", [(out_d[s, t0 + j * 128:t0 + (j + 1) * 128, :], og.ap)],
                      reads=og.res, owner=og.res[0], final=True)

    P.finish()
    P.emit()
    nc._used_weights = list(wd_cache.keys())


_NC_CACHE = {}


def _get_nc(key, cfg):
    if key not in _NC_CACHE:
        _NC_CACHE[key] = build(cfg)
    return _NC_CACHE[key]


def make_gains(inp):
    g = np.zeros((NGV, D), np.float32)
    g[G_MIX:G_MIX + 4] = inp["norm_mix"]
    g[G_XAT:G_XAT + 4] = inp["norm_xattn"]
    g[G_FFN:G_FFN + 4] = inp["norm_ffn"]
    g[G_MEM] = inp["norm_memory"]
    g[G_FIN] = inp["norm_final"]
    g[G_PSC:G_PSC + 2] = inp["pool_scale"]
    return np.ascontiguousarray(g.reshape(NGV, 16, 128).transpose(2, 0, 1).reshape(128, NGV * 16))


def run(inp, cfg, core_ids):
    nc = _get_nc(str(sorted(cfg.items())), cfg)
    gains = make_gains(inp)
    cst = pack_consts()
    in_maps = []
    for ci in core_ids:
        m = {
            "x": np.ascontiguousarray(inp["x"][2 * ci:2 * ci + 2]),
            "mem": np.ascontiguousarray(inp["mem"][2 * ci:2 * ci + 2]),
            "pos": np.ascontiguousarray(np.broadcast_to(inp["positions"][2 * ci:2 * ci + 2].astype(np.int32)[:, None, :], (2, 128, S))),
            "gains": gains, "cst": cst,
        }
        for wn in nc._used_weights:
            base, l = wn.rsplit("_", 1)
            if base == "kiwi":
                w = inp["attn_w_in"][int(l)]
                m[wn] = np.ascontiguousarray(np.concatenate([w[:, 3328:3392], w[:, 3328:3392], w[:, 3392:3408], np.zeros((D, 112), np.float32)], axis=1))
            elif base == "attn_w_in":
                w = np.zeros((D, 3584), np.float32)
                w[:, :3408] = inp["attn_w_in"][int(l)]
                m[wn] = w
            else:
                m[wn] = np.ascontiguousarray(inp[base][int(l)])
        in_maps.append(m)
    res = run_bass_kernel_spmd(nc, in_maps, core_ids=list(range(len(core_ids))))
    return [r["out"] for r in res.results]


def kernel(**inp):
    inp = {k: np.asarray(v) for k, v in inp.items()}
    outs = run(inp, {}, list(range(8)))
    return np.concatenate(outs, axis=0).astype(np.float32)
```
